# Optimizing a Trainium2 kernel written in Bass

```python
import math
import jax, jax.numpy as jnp
from jax import lax
import numpy as np

D_MODEL = 1024
BATCH = 32
SEQ = 2048
DEPTH = 2
DEC_BATCH = 32
DEC_SEQ = 64
PAST_LEN = 4096

CHUNK = 64
QBLOCK = 128
EPS = 1e-6
H_A = 4
DH_A = 64
DV_A = 128
H_B = 8
DH_B = 64
H_IDX = 4
D_IDX = 64
TOPK_MAX = 256
H_C = 4
DK_C = 64
DV_C = 128
W_BR = 512
N_BRANCH = 3
NUM_BUCKETS = 32
MAX_DISTANCE = 128
ROPE_BASE = 10000.0
IN_SIZES = (H_A * 2 * DH_A, H_A * 2 * DH_A, H_A * DV_A, W_BR,
            H_B * DH_B, DH_B, DH_B, H_IDX * D_IDX, D_IDX, H_IDX, W_BR,
            H_C * DK_C, H_C * DK_C, H_C * DV_C, W_BR,
            N_BRANCH * D_MODEL)
D_IN = sum(IN_SIZES)

kernel_name = 'hybrid_streaming_encoder_step'


def rmsnorm(x, g):
    xf = x.astype(jnp.float32)
    xf = xf * lax.rsqrt(jnp.mean(xf * xf, axis=-1, keepdims=True) + EPS)
    return xf.astype(x.dtype) * g


def rotary(x, pos):
    half = x.shape[-1] // 2
    inv = ROPE_BASE ** (-jnp.arange(half, dtype=jnp.float32) / half)
    ang = pos.astype(jnp.float32)[:, None] * inv[None, :]
    cos = jnp.cos(ang)[:, None, :]
    sin = jnp.sin(ang)[:, None, :]
    xf = x.astype(jnp.float32)
    x1, x2 = xf[..., :half], xf[..., half:]
    return jnp.concatenate([x1 * cos - x2 * sin, x1 * sin + x2 * cos], axis=-1).astype(x.dtype)


def t5_bucket(rel):
    nb = NUM_BUCKETS // 2
    max_exact = nb // 2
    ret = jnp.where(rel > 0, nb, 0)
    n = jnp.abs(rel)
    large = max_exact + (jnp.log(jnp.maximum(n, 1).astype(jnp.float32) / max_exact)
                         / math.log(MAX_DISTANCE / max_exact) * (nb - max_exact)).astype(jnp.int32)
    large = jnp.minimum(large, nb - 1)
    return ret + jnp.where(n < max_exact, n, large)


def visible(q_pos, k_pos):
    return k_pos < (q_pos // CHUNK + 1) * CHUNK


def block_map(fn, qs, q_pos):
    T = q_pos.shape[0]
    nb = T // QBLOCK
    blocks = tuple(jnp.moveaxis(a.reshape(a.shape[0], nb, QBLOCK, *a.shape[2:]), 1, 0) for a in qs)
    pos_b = q_pos.reshape(nb, QBLOCK)
    out = lax.map(lambda args: fn(*args), (blocks, pos_b))
    out = jnp.moveaxis(out, 0, 1)
    return out.reshape(out.shape[0], T, *out.shape[3:])


def project(x, pos, norm_g, w_in, a_qk_g, b_qk_g):
    B, T, _ = x.shape
    z = rmsnorm(x, norm_g) @ w_in
    points, acc = [], 0
    for s in IN_SIZES[:-1]:
        acc += s
        points.append(acc)
    aq, ak, av, ag, bq, bk, bv, bqi, bki, bwi, bg, cq, ck, cv, cg, mg = jnp.split(z, points, axis=-1)
    return {
        'aq': rmsnorm(aq.reshape(B, T, H_A, 2, DH_A), a_qk_g[0]),
        'ak': rmsnorm(ak.reshape(B, T, H_A, 2, DH_A), a_qk_g[1]),
        'av': av.reshape(B, T, H_A, DV_A),
        'ag': ag,
        'bq': rmsnorm(bq.reshape(B, T, H_B, DH_B), b_qk_g[0]),
        'bk': rmsnorm(bk, b_qk_g[1]),
        'bv': bv,
        'bqi': bqi.reshape(B, T, H_IDX, D_IDX),
        'bki': bki,
        'bwi': bwi,
        'bg': bg,
        'cq': rotary(cq.reshape(B, T, H_C, DK_C), pos),
        'ck': rotary(ck.reshape(B, T, H_C, DK_C), pos) * DK_C ** -0.5,
        'cv': cv.reshape(B, T, H_C, DV_C),
        'cg': cg,
        'mg': mg,
    }


def diff_attention(q, k, v, q_pos, k_pos, bias_a, lam, lam_init, subln_g):
    B, Tq = q.shape[0], q.shape[1]
    bias = bias_a[t5_bucket(k_pos[None, :] - q_pos[:, None])]
    logits = jnp.einsum('bqhcd,bshcd->bhcqs', q, k).astype(jnp.float32) * DH_A ** -0.5
    logits = logits + jnp.transpose(bias, (2, 0, 1)).astype(jnp.float32)[None, :, None]
    mask = visible(q_pos[:, None], k_pos[None, :])
    logits = jnp.where(mask, logits, -1e30)
    p = jax.nn.softmax(logits, axis=-1)
    a = p[:, :, 0] - lam * p[:, :, 1]
    o = jnp.einsum('bhqs,bshv->bqhv', a.astype(v.dtype), v)
    o = rmsnorm(o, subln_g) * (1.0 - lam_init)
    return o.reshape(B, Tq, H_A * DV_A)


def dsa_attention(q, qi, wi, q_pos, k, v, ki, k_pos, bias_b, topk):
    B, Tq = q.shape[0], q.shape[1]
    mask = visible(q_pos[:, None], k_pos[None, :])
    rel = jax.nn.relu(jnp.einsum('bqhd,bsd->bqhs', qi, ki).astype(jnp.float32))
    score = jnp.einsum('bqh,bqhs->bqs', wi.astype(jnp.float32), rel)
    score = jnp.where(mask[None], score, -jnp.inf)
    _, idx = lax.top_k(score, topk)
    gather = jax.vmap(lambda a, i: a[i])
    k_sel = gather(k, idx)
    v_sel = gather(v, idx)
    sel_pos = k_pos[idx]
    valid = visible(q_pos[None, :, None], sel_pos)
    bias = bias_b[t5_bucket(sel_pos - q_pos[None, :, None])]
    logits = jnp.einsum('bqhd,bqkd->bqhk', q, k_sel).astype(jnp.float32) * DH_B ** -0.5
    logits = logits + jnp.moveaxis(bias, -1, 2).astype(jnp.float32)
    logits = jnp.where(valid[:, :, None, :], logits, -1e30)
    p = jax.nn.softmax(logits, axis=-1)
    o = jnp.einsum('bqhk,bqkd->bqhd', p.astype(v.dtype), v_sel)
    return o.reshape(B, Tq, H_B * DH_B)


def retention_chunk(q, k, v, state, log_gamma):
    C = q.shape[1]
    n = jnp.arange(C, dtype=jnp.float32)
    diff = n[:, None] - n[None, :]
    dmat = jnp.where(diff >= 0, jnp.exp(log_gamma[:, None, None] * jnp.maximum(diff, 0.0)[None]), 0.0)
    inner = jnp.einsum('bqhd,bshd->bhqs', q, k) * dmat[None]
    decay_q = jnp.exp((n[:, None] + 1.0) * log_gamma[None, :])
    o = jnp.einsum('bhqs,bshv->bqhv', inner, v) + jnp.einsum('bqhd,bhdv->bqhv', q * decay_q[None, :, :, None], state)
    decay_k = jnp.exp((C - 1.0 - n)[:, None] * log_gamma[None, :])
    new_state = jnp.exp(C * log_gamma)[None, :, None, None] * state + jnp.einsum('bshd,bshv->bhdv', k * decay_k[None, :, :, None], v)
    return o, new_state


def retention_prompt(q, k, v, log_gamma):
    B, T = q.shape[0], q.shape[1]
    nc = T // CHUNK

    def to_chunks(a):
        return jnp.moveaxis(a.astype(jnp.float32).reshape(B, nc, CHUNK, *a.shape[2:]), 1, 0)

    def step(s, inp):
        o, s = retention_chunk(inp[0], inp[1], inp[2], s, log_gamma)
        return s, o

    s0 = jnp.zeros((B, H_C, DK_C, DV_C), jnp.float32)
    s, o = lax.scan(step, s0, (to_chunks(q), to_chunks(k), to_chunks(v)))
    o = jnp.moveaxis(o, 0, 1).reshape(B, T, H_C, DV_C)
    return o, s


def merge(oa, ob, oc, P, w_branch, w_out):
    B, T, _ = oa.shape
    br = jnp.stack([oa * jax.nn.silu(P['ag']), ob * jax.nn.silu(P['bg']), oc * jax.nn.silu(P['cg'])], axis=2)
    proj = jnp.einsum('btnw,nwd->btnd', br, w_branch)
    gates = jax.nn.sigmoid(P['mg'].reshape(B, T, N_BRANCH, D_MODEL))
    return jnp.sum(gates * proj, axis=2) @ w_out


def setup_inputs(seed: int = 0) -> dict:
    key = jax.random.key(seed)
    ks = jax.random.split(key, 18)
    f32 = jnp.float32

    def nrm(k, shape, s):
        return jax.random.normal(k, shape, f32) * s

    return {
        'x_prompt': nrm(ks[0], (BATCH, SEQ, D_MODEL), 1.0),
        'x_sample': nrm(ks[1], (DEC_BATCH, DEC_SEQ, D_MODEL), 1.0),
        'cache_a_k': nrm(ks[2], (DEPTH, DEC_BATCH, PAST_LEN, H_A, 2, DH_A), 1.0),
        'cache_a_v': nrm(ks[3], (DEPTH, DEC_BATCH, PAST_LEN, H_A, DV_A), 1.0),
        'cache_b_k': nrm(ks[4], (DEPTH, DEC_BATCH, PAST_LEN, DH_B), 1.0),
        'cache_b_v': nrm(ks[5], (DEPTH, DEC_BATCH, PAST_LEN, DH_B), 1.0),
        'cache_b_kidx': nrm(ks[6], (DEPTH, DEC_BATCH, PAST_LEN, D_IDX), 1.0),
        'state_c': nrm(ks[7], (DEPTH, DEC_BATCH, H_C, DK_C, DV_C), 0.5),
        'rel_bias': nrm(ks[8], (NUM_BUCKETS, H_A + H_B), 0.5),
        'norm_g': 1.0 + nrm(ks[9], (DEPTH, D_MODEL), 0.02),
        'w_in': nrm(ks[10], (DEPTH, D_MODEL, D_IN), D_MODEL ** -0.5),
        'a_qk_g': 1.0 + nrm(ks[11], (DEPTH, 2, DH_A), 0.02),
        'a_lambda': nrm(ks[12], (DEPTH, 4, DH_A), 0.1),
        'a_subln_g': 1.0 + nrm(ks[13], (DEPTH, DV_A), 0.02),
        'b_qk_g': 1.0 + nrm(ks[14], (DEPTH, 2, DH_B), 0.02),
        'c_gn_g': 1.0 + nrm(ks[15], (DEPTH, DV_C), 0.02),
        'w_branch': nrm(ks[16], (DEPTH, N_BRANCH, W_BR, D_MODEL), W_BR ** -0.5),
        'w_out': nrm(ks[17], (DEPTH, D_MODEL, D_MODEL), D_MODEL ** -0.5),
    }


def reference(x_prompt, x_sample, cache_a_k, cache_a_v, cache_b_k, cache_b_v, cache_b_kidx, state_c,
              rel_bias, norm_g, w_in, a_qk_g, a_lambda, a_subln_g, b_qk_g, c_gn_g, w_branch, w_out):
    Bp, Tp, _ = x_prompt.shape
    Bs, Ts, _ = x_sample.shape
    past = cache_a_k.shape[2]
    pos_p = jnp.arange(Tp, dtype=jnp.int32)
    pos_s = past + jnp.arange(Ts, dtype=jnp.int32)
    kpos_s = jnp.arange(past + Ts, dtype=jnp.int32)
    topk_p = min(TOPK_MAX, Tp // 4)
    topk_s = min(TOPK_MAX, (past + Ts) // 4)
    bias_a = rel_bias[:, :H_A]
    bias_b = rel_bias[:, H_A:]
    log_gamma = jnp.log(1.0 - 2.0 ** (-5.0 - jnp.arange(H_C, dtype=jnp.float32)))

    yp, ys = x_prompt, x_sample
    pak, pav, pbk, pbv, pbi, psc = [], [], [], [], [], []
    sak, sav, sbk, sbv, sbi, ssc = [], [], [], [], [], []
    for l in range(DEPTH):
        lam_init = 0.8 - 0.6 * math.exp(-0.3 * l)
        lp = a_lambda[l].astype(jnp.float32)
        lam = jnp.exp(jnp.sum(lp[0] * lp[1])) - jnp.exp(jnp.sum(lp[2] * lp[3])) + lam_init

        P = project(yp, pos_p, norm_g[l], w_in[l], a_qk_g[l], b_qk_g[l])
        oa = block_map(lambda qs, qp: diff_attention(qs[0], P['ak'], P['av'], qp, pos_p, bias_a, lam, lam_init, a_subln_g[l]),
                       (P['aq'],), pos_p)
        ob = block_map(lambda qs, qp: dsa_attention(qs[0], qs[1], qs[2], qp, P['bk'], P['bv'], P['bki'], pos_p, bias_b, topk_p),
                       (P['bq'], P['bqi'], P['bwi']), pos_p)
        oc, sc = retention_prompt(P['cq'], P['ck'], P['cv'], log_gamma)
        oc = rmsnorm(oc.astype(yp.dtype), c_gn_g[l]).reshape(Bp, Tp, W_BR)
        yp = yp + merge(oa, ob, oc, P, w_branch[l], w_out[l])
        pak.append(P['ak'])
        pav.append(P['av'])
        pbk.append(P['bk'])
        pbv.append(P['bv'])
        pbi.append(P['bki'])
        psc.append(sc.astype(x_prompt.dtype))

        S = project(ys, pos_s, norm_g[l], w_in[l], a_qk_g[l], b_qk_g[l])
        ka = jnp.concatenate([cache_a_k[l], S['ak']], axis=1)
        va = jnp.concatenate([cache_a_v[l], S['av']], axis=1)
        kb = jnp.concatenate([cache_b_k[l], S['bk']], axis=1)
        vb = jnp.concatenate([cache_b_v[l], S['bv']], axis=1)
        kib = jnp.concatenate([cache_b_kidx[l], S['bki']], axis=1)
        oa = diff_attention(S['aq'], ka, va, pos_s, kpos_s, bias_a, lam, lam_init, a_subln_g[l])
        ob = dsa_attention(S['bq'], S['bqi'], S['bwi'], pos_s, kb, vb, kib, kpos_s, bias_b, topk_s)
        oc, ss = retention_chunk(S['cq'].astype(jnp.float32), S['ck'].astype(jnp.float32), S['cv'].astype(jnp.float32),
                                 state_c[l].astype(jnp.float32), log_gamma)
        oc = rmsnorm(oc.astype(ys.dtype), c_gn_g[l]).reshape(Bs, Ts, W_BR)
        ys = ys + merge(oa, ob, oc, S, w_branch[l], w_out[l])
        sak.append(S['ak'])
        sav.append(S['av'])
        sbk.append(S['bk'])
        sbv.append(S['bv'])
        sbi.append(S['bki'])
        ssc.append(ss.astype(state_c.dtype))

    return (yp, ys,
            jnp.stack(pak), jnp.stack(pav), jnp.stack(pbk), jnp.stack(pbv), jnp.stack(pbi), jnp.stack(psc),
            jnp.stack(sak), jnp.stack(sav), jnp.stack(sbk), jnp.stack(sbv), jnp.stack(sbi), jnp.stack(ssc))
```

```python
import math
from contextlib import ExitStack
import numpy as np
import ml_dtypes
import concourse.bass as bass
import concourse.mybir as mybir
from concourse.bass_utils import run_bass_kernel_spmd

F32 = mybir.dt.float32
BF16 = mybir.dt.bfloat16
ALU = mybir.AluOpType
AF = mybir.ActivationFunctionType
AX = mybir.AxisListType

ENGS = ("pe", "act", "dve", "pool", "sp")
NEG = -3.0e38
D = 1024
EPS = 1e-6
NGRP = 17
G_AQ, G_AK, G_AV, G_AG, G_BQ, G_BM1, G_BM2, G_BG, G_CQK, G_CV, G_CG, G_MG0 = 0, 1, 2, 3, 4, 5, 6, 7, 8, 9, 10, 11
GCOLS = [512] * NGRP
GCOLS[G_BM2] = 128


class Cfg:
    def __init__(self, NBP=4, S=2048, NBS=4, PAST=4096, debug=False):
        self.NBP, self.S, self.NBS, self.PAST = NBP, S, NBS, PAST
        self.NT = S // 128
        self.NBC = PAST // 128
        self.KP = min(256, S // 4)
        self.KS = min(256, (PAST + 64) // 4)
        self.debug = debug
        self.stop = None
        self.lvl = 9


class Prog:
    def __init__(self, nc):
        self.nc = nc
        self.ops = {e: [] for e in ENGS}
        self.cnt = {}
        self.waited = {e: {} for e in ENGS}
        self.pending = {e: {} for e in ENGS}
        self.last_w = {}
        self.readers = {}
        self.extra = {}
        self.n_dma_sem = 0

    def fence(self, src_keys, dst_keys):
        evs = {}
        for k in src_keys:
            w = self.last_w.get(k)
            if w is not None and evs.get(w[0], 0) < w[1]:
                evs[w[0]] = w[1]
            for sk, v in self.readers.get(k, {}).items():
                if evs.get(sk, 0) < v:
                    evs[sk] = v
        for k in dst_keys:
            self.extra[k] = list(evs.items())

    def new_dma_sem(self):
        k = ("dma", self.n_dma_sem)
        self.n_dma_sem += 1
        self.cnt[k] = 0
        return k

    def barrier(self):
        for e in ENGS:
            for sk, v in self.cnt.items():
                if v > 0 and self.waited[e].get(sk, 0) < v:
                    self.pending[e][sk] = v

    def _need(self, eng, waits, ev, same_ok):
        sk, val = ev
        if sk == ("eng", eng) and same_ok:
            return
        if self.waited[eng].get(sk, 0) >= val:
            return
        if waits.get(sk, 0) < val:
            waits[sk] = val

    def _deps(self, eng, reads, writes, is_dma):
        waits = {}
        if self.pending[eng]:
            for sk, v in self.pending[eng].items():
                self._need(eng, waits, (sk, v), False)
            self.pending[eng] = {}
        if self.extra:
            for k in list(reads) + list(writes):
                evs = self.extra.get(k)
                if evs:
                    for ev in evs:
                        self._need(eng, waits, ev, False)
        for k in reads:
            w = self.last_w.get(k)
            if w is not None:
                self._need(eng, waits, w, False)
        for k in writes:
            w = self.last_w.get(k)
            if w is not None:
                self._need(eng, waits, w, not is_dma)
            for sk, val in self.readers.get(k, {}).items():
                self._need(eng, waits, (sk, val), not is_dma)
        for sk, val in waits.items():
            self.waited[eng][sk] = val
        return list(waits.items())

    def _commit(self, ev, reads, writes):
        for k in writes:
            self.last_w[k] = ev
            self.readers[k] = {}
        for k in reads:
            d = self.readers.setdefault(k, {})
            if d.get(ev[0], 0) < ev[1]:
                d[ev[0]] = ev[1]

    def op(self, eng, reads, writes, fn):
        waits = self._deps(eng, reads, writes, False)
        sk = ("eng", eng)
        self.cnt[sk] = self.cnt.get(sk, 0) + 1
        self.ops[eng].append((waits, fn, sk, 1))
        self._commit((sk, self.cnt[sk]), reads, writes)

    def dma(self, queue, sem, reads, writes, fn):
        waits = self._deps(queue, reads, writes, True)
        self.cnt[sem] += 16
        self.ops[queue].append((waits, fn, sem, 16))
        self._commit((sem, self.cnt[sem]), reads, writes)

    def emit(self, stack):
        nc = self.nc
        handles = {}
        for sk in self.cnt:
            handles[sk] = stack.enter_context(nc.semaphore("s_%s_%s" % sk))
        fin = [(sk, v) for sk, v in self.cnt.items() if v > 0]
        block = stack.enter_context(nc.Block())
        ops = self.ops

        def run(engine, name, extra=None):
            for waits, fn, sk, inc in ops[name]:
                for wk, wv in waits:
                    engine.wait_ge(handles[wk], wv)
                ins = fn(engine)
                ins.then_inc(handles[sk], inc)
            if extra:
                for wk, wv in extra:
                    engine.wait_ge(handles[wk], wv)

        @block.sync
        def _(e):
            run(e, "sp", fin)

        @block.tensor
        def _(e):
            run(e, "pe")

        @block.scalar
        def _(e):
            run(e, "act")

        @block.vector
        def _(e):
            run(e, "dve")

        @block.gpsimd
        def _(e):
            run(e, "pool")


class Arena:
    def __init__(self, t, nbytes):
        self.t = t
        self.nbytes = nbytes
        self.off = 0
        self.peak = 0

    def reset(self):
        self.off = 0

    def alloc(self, free_shape, dt):
        n = 1
        for s in free_shape:
            n *= s
        nb = n * (4 if dt == F32 else 2)
        nb = (nb + 63) // 64 * 64
        assert self.off + nb <= self.nbytes, ("arena overflow", self.off, nb, self.nbytes)
        ap = self.t[:, self.off // 2:(self.off + nb) // 2]
        self.off += nb
        self.peak = max(self.peak, self.off)
        if dt == F32:
            ap = ap.bitcast(F32)
        ap = ap[:, 0:n]
        if len(free_shape) > 1:
            names = " ".join("d%d" % i for i in range(len(free_shape)))
            kw = {"d%d" % i: free_shape[i] for i in range(1, len(free_shape))}
            ap = ap.rearrange("p (%s) -> p %s" % (names, names), **kw)
        return ap


def t5_bucket_np(rel):
    nb = 16
    max_exact = 8
    ret = np.where(rel > 0, nb, 0)
    n = np.abs(rel)
    large = max_exact + (np.log(np.maximum(n, 1).astype(np.float32) / max_exact)
                         / np.float32(math.log(128 / max_exact)) * (nb - max_exact)).astype(np.int32)
    large = np.minimum(large, nb - 1)
    return ret + np.where(n < max_exact, n, large)


def make_consts(cfg):
    c = {}
    c["identb"] = np.eye(128, dtype=np.float32).astype(ml_dtypes.bfloat16)
    c["identf"] = np.eye(128, dtype=np.float32)
    c["Jm"] = np.eye(128, dtype=np.float32)[::-1].copy()
    rel = np.arange(384, dtype=np.int64) - 255
    bk = t5_bucket_np(rel.astype(np.int32))
    oh = np.zeros((32, 384), np.float32)
    oh[bk, np.arange(384)] += 1.0
    oh[15, :] -= 1.0
    oh[:, 383] = 0.0
    c["ohd"] = oh
    half = 32
    inv = (10000.0 ** (-np.arange(half, dtype=np.float32) / np.float32(half))).astype(np.float32)
    posP = (np.arange(cfg.NT)[None, :] * 128 + np.arange(128)[:, None]).astype(np.float32)
    angP = posP[:, :, None] * inv[None, None, :]
    c["cosP"] = np.cos(angP).astype(np.float32)
    c["sinP"] = np.sin(angP).astype(np.float32)
    posS = (cfg.PAST + np.arange(64)).astype(np.float32)
    angS = posS[:, None] * inv[None, :]
    c["cosS"] = np.cos(angS).astype(np.float32)
    c["sinS"] = np.sin(angS).astype(np.float32)
    gam = 1.0 - 2.0 ** (-5.0 - np.arange(4, dtype=np.float64))
    n = np.arange(128, dtype=np.float64)
    c["dq"] = (gam[None, :] ** (n[:, None] + 1.0)).astype(np.float32)
    c["dk128"] = (0.125 * gam[None, :] ** (127.0 - n[:, None])).astype(np.float32)
    c["dk64"] = (0.125 * gam[None, :] ** (63.0 - n[:64, None])).astype(np.float32)
    diff = n[None, :] - n[:, None]
    DT = np.where(diff[:, None, :] >= 0, 0.125 * gam[None, :, None] ** np.maximum(diff, 0.0)[:, None, :], 0.0)
    c["DT"] = DT.astype(np.float32)
    return c, gam


CONST_SPECS = lambda cfg: {
    "identb": ([128, 128], BF16), "identf": ([128, 128], F32), "Jm": ([128, 128], F32), "ohd": ([32, 384], F32),
    "cosP": ([128, cfg.NT, 32], F32), "sinP": ([128, cfg.NT, 32], F32), "cosS": ([64, 32], F32), "sinS": ([64, 32], F32),
    "dq": ([128, 4], F32), "dk128": ([128, 4], F32), "dk64": ([64, 4], F32), "DT": ([128, 4, 128], F32),
}


def build(cfg):
    nc = bass.Bass("TRN2", target_bir_lowering=False)
    NBP, S, NBS, PAST, NT, NBC = cfg.NBP, cfg.S, cfg.NBS, cfg.PAST, cfg.NT, cfg.NBC
    _, gam = make_consts(cfg)

    def din(name, shape, dt=F32):
        return nc.dram_tensor(name, list(shape), dt, kind="ExternalInput").ap()

    def dout(name, shape, dt=F32):
        return nc.dram_tensor(name, list(shape), dt, kind="ExternalOutput").ap()

    def dint(name, shape, dt=F32):
        return nc.dram_tensor(name, list(shape), dt, kind="Internal").ap()

    xp = din("xp", [max(NBP, 1), S, D])
    xs = din("xs", [max(NBS, 1), 64, D])
    cak = din("cak", [2, max(NBS, 1), PAST, 512])
    cav = din("cav", [2, max(NBS, 1), PAST, 512])
    cbk = din("cbk", [2, max(NBS, 1), PAST, 64])
    cbv = din("cbv", [2, max(NBS, 1), PAST, 64])
    cbi = din("cbi", [2, max(NBS, 1), PAST, 64])
    stc = din("stc", [2, max(NBS, 1), 4, 64, 128])
    relb = din("relb", [32, 12])
    ngT = din("ngT", [2, 128, 8])
    wing = din("wing", [2, NGRP, D, 512])
    aqkg = din("aqkg", [2, 2, 64])
    alam = din("alam", [2, 4, 64])
    asub = din("asub", [2, 128])
    bqkg = din("bqkg", [2, 2, 64])
    cgn = din("cgn", [2, 128])
    wbr = din("wbr", [2, 3, 512, D])
    wout = din("wout", [2, D, D])
    cd = {k: din("c_" + k, shp, dt) for k, (shp, dt) in CONST_SPECS(cfg).items()}

    yp = dout("yp", [max(NBP, 1), S, D])
    ys = dout("ys", [max(NBS, 1), 64, D])
    akp = dout("akp", [2, max(NBP, 1), S, 512])
    avp = dout("avp", [2, max(NBP, 1), S, 512])
    bkp = dout("bkp", [2, max(NBP, 1), S, 64])
    bvp = dout("bvp", [2, max(NBP, 1), S, 64])
    bip = dout("bip", [2, max(NBP, 1), S, 64])
    scp = dout("scp", [2, max(NBP, 1), 4, 64, 128])
    aks = dout("aks", [2, max(NBS, 1), 64, 512])
    avs = dout("avs", [2, max(NBS, 1), 64, 512])
    bks = dout("bks", [2, max(NBS, 1), 64, 64])
    bvs = dout("bvs", [2, max(NBS, 1), 64, 64])
    bis = dout("bis", [2, max(NBS, 1), 64, 64])
    scs = dout("scs", [2, max(NBS, 1), 4, 64, 128])

    Wb = dint("Wb", [2, NGRP, 128, 4096], BF16)
    Wbr = dint("Wbr", [2, 6, 128, 2048], BF16)
    Wo = dint("Wo", [2, 2, 128, 4096], BF16)
    ybuf = dint("ybuf", [S, D])
    vecd = dint("vecd", [12, 384])

    P = Prog(nc)
    dbg_outs = {}

    with ExitStack() as st:
        def sb(name, shape, dt):
            return st.enter_context(nc.sbuf_tensor(name, list(shape), dt))

        identb = sb("identb", [128, 128], BF16)
        identf = sb("identf", [128, 128], F32)
        Jm = sb("Jm", [128, 128], F32)
        ohd = sb("ohd", [32, 384], F32)
        cosP = sb("cosP", [128, NT, 32], F32)
        sinP = sb("sinP", [128, NT, 32], F32)
        cosS = sb("cosS", [64, 32], F32)
        sinS = sb("sinS", [64, 32], F32)
        dq = sb("dq", [128, 4], F32)
        dk128 = sb("dk128", [128, 4], F32)
        dk64 = sb("dk64", [64, 4], F32)
        DT = sb("DT", [128, 4, 128], F32)
        csb = {"identb": identb, "identf": identf, "Jm": Jm, "ohd": ohd, "cosP": cosP, "sinP": sinP, "cosS": cosS,
               "sinS": sinS, "dq": dq, "dk128": dk128, "dk64": dk64, "DT": DT}
        biasT = sb("biasT", [128, 2, 12, 128], BF16)
        rb32 = sb("rb32", [32, 12], F32)
        rbB = sb("rbB", [128, 32, 12], F32)
        bmax = sb("bmax", [128, 12], F32)
        vec_sb = sb("vec_sb", [12, 384], F32)
        gq_a = sb("gq_a", [128, 2, 64], F32)
        gk_a = sb("gk_a", [128, 2, 64], F32)
        gq_b = sb("gq_b", [128, 2, 64], F32)
        gk_b = sb("gk_b", [128, 2, 64], F32)
        subln = sb("subln", [128, 2, 128], F32)
        gn_c = sb("gn_c", [128, 2, 128], F32)
        farb = sb("farb", [128, 2, 12], F32)
        neglam = sb("neglam", [128, 2], F32)
        ngs = sb("ngs", [128, 2, 8], F32)
        lpb = sb("lpb", [128, 4, 64], F32)
        sm = sb("sm", [128, 64], F32)
        epsc = sb("epsc", [128, 1], F32)
        NWS = 2
        wslot = [sb("wslot%d" % i, [128, 8, 512], BF16) for i in range(NWS)]
        NPT = 4
        PT = [sb("PT%d" % i, [128, 512], BF16) for i in range(NPT)]
        BK = [st.enter_context(nc.psum_tensor("bk%d" % i, [128, 512], F32)) for i in range(8)]
        pTb = BK[6][:].bitcast(BF16)
        ARENA_BYTES = nc.sbuf_bytes_remaining - 1024
        ARENA_BYTES = ARENA_BYTES // 64 * 64
        arena_t = sb("arena", [128, ARENA_BYTES // 2], BF16)
        AR = Arena(arena_t, ARENA_BYTES)

        ctr = {"w": 0, "sq": 0, "bank": 0, "pt": 0, "sm": 0}
        named_sems = {}

        def sem_for(name):
            name = str(name)
            for pre in ("c_", "gq_", "gk_", "subln", "gn_c", "cob", "hk"):
                if name[1:].startswith(pre):
                    name = name[0] + pre
            if name not in named_sems:
                named_sems[name] = P.new_dma_sem()
            return named_sems[name]

        def dma_in(reads, writes, out_ap, in_ap, queue="sp"):
            P.dma(queue, sem_for("L" + str(writes[0])), reads, writes, lambda e: e.dma_start(out=out_ap, in_=in_ap))

        def dma_out(reads, writes, out_ap, in_ap, queue="pool"):
            P.dma(queue, sem_for("S" + str(reads[0])), reads, writes, lambda e: e.dma_start(out=out_ap, in_=in_ap))

        def smcol(n=1):
            if ctr["sm"] + n > 64:
                ctr["sm"] = 0
            a = ctr["sm"]
            ctr["sm"] += n
            return sm[:, a:a + n], ["sm%d" % i for i in range(a, a + n)]

        def rstd_from_ss(Pn, ss_ap, ss_keys, scale):
            P.op("act", ss_keys + ["epsc"], ss_keys, lambda e: e.activation(out=ss_ap, in_=ss_ap, func=AF.Ln, scale=scale, bias=epsc[0:Pn, :]))
            P.op("act", ss_keys, ss_keys, lambda e: e.activation(out=ss_ap, in_=ss_ap, func=AF.Exp, scale=-0.5))

        P.op("pool", [], ["epsc"], lambda e: e.memset(epsc[:], EPS))
        for k, t in csb.items():
            dma_in([], ["c_" + k], t[:], cd[k])
        dma_in([], ["rb32"], rb32[:], relb[:, :])
        dma_in([], ["rbB"], rbB[:].rearrange("p b h -> p (b h)"),
               relb.rearrange("b h -> (b h)").unsqueeze(0).partition_broadcast(128))
        dma_in([], ["ngs"], ngs[:].rearrange("p l c -> p l c"), ngT.rearrange("l k c -> k l c"))
        for l in range(2):
            for (dst, src, nm) in ((gq_a, aqkg[l, 0:1, :], "gq_a"), (gk_a, aqkg[l, 1:2, :], "gk_a"),
                                   (gq_b, bqkg[l, 0:1, :], "gq_b"), (gk_b, bqkg[l, 1:2, :], "gk_b")):
                dma_in([], [nm + str(l)], dst[:, l, :], src.partition_broadcast(128))
            dma_in([], ["subln%d" % l], subln[:, l, :], asub[l:l + 1, :].partition_broadcast(128))
            dma_in([], ["gn_c%d" % l], gn_c[:, l, :], cgn[l:l + 1, :].partition_broadcast(128))

        stop_lvl = {'s1': 1, 's2': 2, 's3': 3}.get(cfg.stop, 9)
        if stop_lvl >= 2:
            P.op("pe", ["rb32", "c_ohd"], ["bk7"], lambda e: e.matmul(BK[7][0:12, 0:384], lhsT=rb32[:], rhs=ohd[:], start=True, stop=True))
            P.op("act", ["bk7"], ["vec_sb"], lambda e: e.activation(out=vec_sb[:], in_=BK[7][0:12, 0:384], func=AF.Copy, scale=8.0))
            dma_out(["vec_sb"], ["vecd"], vecd[:, :], vec_sb[:])
        AR.reset()
        hk = [AR.alloc([128], F32) for _ in range(2)]
        tmpf = [AR.alloc([128], F32) for _ in range(2)]
        n_t = 0
        for kind in (range(2) if stop_lvl >= 2 else []):
            base = 128 if kind == 0 else 0
            for h in range(12):
                i = n_t % 2
                n_t += 1
                src = bass.AP(vecd.tensor, h * 384 + base, [[1, 128], [1, 128]])
                dma_in(["vecd"], ["hk%d" % i], hk[i], src)
                P.op("pe", ["hk%d" % i, "c_Jm"], ["bk7"], (lambda i: lambda e: e.matmul(BK[7][:, 0:128], lhsT=hk[i], rhs=Jm[:], start=True, stop=True))(i))
                P.op("act", ["bk7"], ["tmpf%d" % i], (lambda i: lambda e: e.activation(out=tmpf[i], in_=BK[7][:, 0:128], func=AF.Copy))(i))
                if kind == 0:
                    P.op("dve", ["tmpf%d" % i], ["tmpf%d" % i], (lambda i: lambda e: e.tensor_scalar(
                        out=tmpf[i][64:128, 0:64], in0=tmpf[i][64:128, 0:64], scalar1=-80000.0, scalar2=None, op0=ALU.add))(i))
                P.op("dve", ["tmpf%d" % i], ["biasT"], (lambda i, kind, h: lambda e: e.tensor_copy(out=biasT[:, kind, h, :], in_=tmpf[i]))(i, kind, h))

        if stop_lvl >= 3:
            P.op("dve", ["rbB"], ["bmax"], lambda e: e.tensor_reduce(out=bmax[:], in_=rbB[:].rearrange("p b h -> p h b"), axis=AX.X, op=ALU.max))
        for l in (range(2) if stop_lvl >= 3 else []):
            lam_init = 0.8 - 0.6 * math.exp(-0.3 * l)
            for (gq, gk, nq, nk_, h0, h1) in ((gq_a, gk_a, "gq_a", "gk_a", 0, 4), (gq_b, gk_b, "gq_b", "gk_b", 4, 12)):
                c1, k1 = smcol(2)
                tq = AR.alloc([64], F32)
                P.op("dve", [nq + str(l)], ["tq"], lambda e, gq=gq, tq=tq, l=l: e.scalar_tensor_tensor(out=tq, in0=gq[:, l, :], scalar=-1.0, in1=gq[:, l, :], op0=ALU.mult, op1=ALU.max))
                P.op("dve", ["tq"], [k1[0]], lambda e, tq=tq, c1=c1: e.tensor_reduce(out=c1[:, 0:1], in_=tq, axis=AX.X, op=ALU.max))
                P.op("dve", [nk_ + str(l), k1[0]], ["tq"], lambda e, gk=gk, tq=tq, l=l: e.scalar_tensor_tensor(out=tq, in0=gk[:, l, :], scalar=-1.0, in1=gk[:, l, :], op0=ALU.mult, op1=ALU.max))
                P.op("dve", ["tq"], [k1[1]], lambda e, tq=tq, c1=c1: e.tensor_reduce(out=c1[:, 1:2], in_=tq, axis=AX.X, op=ALU.max))
                P.op("dve", k1, [k1[0]], lambda e, c1=c1: e.scalar_tensor_tensor(out=c1[:, 0:1], in0=c1[:, 0:1], scalar=8.0, in1=c1[:, 1:2], op0=ALU.mult, op1=ALU.mult))
                P.op("dve", ["rbB", "bmax"], ["farb"], lambda e, h0=h0, h1=h1, l=l: e.tensor_tensor(out=farb[:, l, h0:h1], in0=rbB[:, 15, h0:h1], in1=bmax[:, h0:h1], op=ALU.subtract))
                P.op("dve", ["farb", k1[0]], ["farb"], lambda e, h0=h0, h1=h1, c1=c1, l=l: e.tensor_scalar(out=farb[:, l, h0:h1], in0=farb[:, l, h0:h1], scalar1=c1[:, 0:1], scalar2=None, op0=ALU.subtract))
            dma_in([], ["lpb"], lpb[:].rearrange("p a d -> p (a d)"), alam[l].rearrange("a d -> (a d)").unsqueeze(0).partition_broadcast(128))
            c2, k2 = smcol(2)
            tq2 = AR.alloc([64], F32)
            for j in range(2):
                P.op("dve", ["lpb"], ["tq2"], lambda e, j=j, tq2=tq2: e.tensor_tensor(out=tq2, in0=lpb[:, 2 * j, :], in1=lpb[:, 2 * j + 1, :], op=ALU.mult))
                P.op("dve", ["tq2"], [k2[j]], lambda e, j=j, tq2=tq2, c2=c2: e.tensor_reduce(out=c2[:, j:j + 1], in_=tq2, axis=AX.X, op=ALU.add))
            P.op("act", k2, k2, lambda e, c2=c2: e.activation(out=c2, in_=c2, func=AF.Exp))
            P.op("dve", k2, ["neglam"], lambda e, c2=c2, lam_init=lam_init, l=l: e.scalar_tensor_tensor(
                out=neglam[:, l:l + 1], in0=c2[:, 1:2], scalar=-lam_init, in1=c2[:, 0:1], op0=ALU.add, op1=ALU.subtract))
            P.op("dve", ["subln%d" % l], ["subln%d" % l], lambda e, lam_init=lam_init, l=l: e.tensor_scalar(
                out=subln[:, l, :], in0=subln[:, l, :], scalar1=1.0 - lam_init, scalar2=None, op0=ALU.mult))

        P.barrier()
        do_conv = cfg.stop not in ('setup', 's1', 's2', 's3')
        AR.reset()
        cst = [AR.alloc([8, 512], F32) for _ in range(2)]
        cob = [AR.alloc([8, 512], BF16) for _ in range(2)]
        engs3 = ("act", "dve", "pool")
        nconv = 0

        def conv_chunk(i, kc, ncol, scale_ap, tag):
            eng = engs3[(nconv * 8 + kc) % 3]
            o = cob[i][:, kc, 0:ncol]
            s_ = cst[i][:, kc, 0:ncol]
            rk = ["cst%d" % i] + (["ngs"] if scale_ap is not None else [])
            wk = ["cob%d_%d" % (i, kc)]
            if eng == "act":
                if scale_ap is not None:
                    P.op("act", rk, wk, lambda e: e.activation(out=o, in_=s_, func=AF.Copy, scale=scale_ap))
                else:
                    P.op("act", rk, wk, lambda e: e.activation(out=o, in_=s_, func=AF.Copy))
            else:
                if scale_ap is not None:
                    P.op(eng, rk, wk, lambda e: e.tensor_scalar(out=o, in0=s_, scalar1=scale_ap, scalar2=None, op0=ALU.mult))
                else:
                    P.op(eng, rk, wk, lambda e: e.tensor_copy(out=o, in_=s_))

        for l in (range(2) if do_conv else []):
            for g in range(NGRP):
                i = nconv % 2
                ncol = GCOLS[g]
                dma_in([], ["cst%d" % i], cst[i][:, :, 0:ncol], wing[l, g, :, 0:ncol].rearrange("(c k) n -> k c n", k=128))
                for kc in range(8):
                    conv_chunk(i, kc, ncol, ngs[:, l, kc:kc + 1], "wi")
                dma_out(["cob%d_%d" % (i, kc) for kc in range(8)], [("Wb", l, g)],
                        Wb[l, g].rearrange("k (c n) -> k c n", n=512)[:, :, 0:ncol], cob[i][:, :, 0:ncol])
                nconv += 1
            for n in range(3):
                for hf in range(2):
                    i = nconv % 2
                    dma_in([], ["cst%d" % i], cst[i][:, 0:4, :], wbr[l, n, :, hf * 512:(hf + 1) * 512].rearrange("(c k) n -> k c n", k=128))
                    for kc in range(4):
                        conv_chunk(i, kc, 512, None, "wb")
                    dma_out(["cob%d_%d" % (i, kc) for kc in range(4)], [("Wbr", l, n * 2 + hf)],
                            Wbr[l, n * 2 + hf].rearrange("k (c n) -> k c n", n=512), cob[i][:, 0:4, :])
                    nconv += 1
            for hf in range(2):
                i = nconv % 2
                dma_in([], ["cst%d" % i], cst[i][:, :, :], wout[l, :, hf * 512:(hf + 1) * 512].rearrange("(c k) n -> k c n", k=128))
                for kc in range(8):
                    conv_chunk(i, kc, 512, None, "wo")
                dma_out(["cob%d_%d" % (i, kc) for kc in range(8)], [("Wo", l, hf)],
                        Wo[l, hf].rearrange("k (c n) -> k c n", n=512), cob[i][:, :, :])
                nconv += 1

        def wload(kind, l, idx):
            i = ctr["w"] % NWS
            ctr["w"] += 1
            if kind == "in":
                ncol = GCOLS[idx]
                dma_in([("Wb", l, idx)], ["wslot%d" % i], wslot[i][:, :, 0:ncol], Wb[l, idx].rearrange("k (c n) -> k c n", n=512)[:, :, 0:ncol])
            elif kind == "br":
                dma_in([("Wbr", l, idx)], ["wslot%d" % i], wslot[i][:, 0:4, :], Wbr[l, idx].rearrange("k (c n) -> k c n", n=512))
            else:
                dma_in([("Wo", l, idx)], ["wslot%d" % i], wslot[i][:, :, :], Wo[l, idx].rearrange("k (c n) -> k c n", n=512))
            return i

        def run_phase(ph):
            prompt = ph == "p"
            Pn = 128 if prompt else 64
            NQ = 512 if prompt else 64
            nsub = NQ // Pn
            NKEY = S if prompt else PAST + 64
            NB = (NKEY + 127) // 128
            TOPK = cfg.KP if prompt else cfg.KS
            nseq = NBP if prompt else NBS
            if nseq == 0:
                return
            P.barrier()
            AR.reset()
            HS = 4 if prompt else 1
            akT = AR.alloc([HS, NB * 128], BF16)
            av_sb = AR.alloc([NB, HS, 129], BF16)
            bkT = AR.alloc([NB * 128], BF16)
            bkiT = AR.alloc([NB * 128], BF16)
            bv_sb = AR.alloc([NB, 65], BF16)
            xnT = AR.alloc([8, NQ], BF16)
            xin = [AR.alloc([D], F32) for _ in range(2)]
            xnb = AR.alloc([D], BF16)
            junk2 = [AR.alloc([512], F32) for _ in range(2)]
            aqT = AR.alloc([4, NQ], BF16)
            bqT = AR.alloc([4, NQ], BF16)
            bqiT = AR.alloc([2, NQ], BF16)
            gates = [AR.alloc([nsub, 512], BF16) for _ in range(3)]
            bwi = AR.alloc([nsub, 4], F32)
            state = AR.alloc([4, 128], F32)
            brT = AR.alloc([3, 4, NQ], BF16)
            brX = AR.alloc([nsub, 512], BF16)
            stg = [AR.alloc([512], F32) for _ in range(2)]
            tA = [AR.alloc([128], F32) for _ in range(2)]
            qn = [AR.alloc([512], BF16) for _ in range(2)]
            t12 = [AR.alloc([512], F32) for _ in range(2)]
            m8 = AR.alloc([8], F32)
            rtmp = [AR.alloc([8, 32], F32) for _ in range(2)]
            cqd = AR.alloc([256], F32)
            ckd = AR.alloc([256], F32)
            cT = [AR.alloc([2, 128], F32) for _ in range(3)]
            innerTm = [AR.alloc([128], F32) for _ in range(2)]
            rotc1 = AR.alloc([512], F32)
            cvs1 = AR.alloc([512], F32)
            rotc = [rotc1 for _ in range(nsub)]
            cvs = [cvs1 for _ in range(nsub)]
            rl = [AR.alloc([512], F32) for _ in range(2)]
            fz = [AR.alloc([8], F32) for _ in range(4)]
            tB = [AR.alloc([128], F32) for _ in range(2)]
            reg0 = AR.off
            scores = AR.alloc([NB * 128], F32)
            nmrow = AR.alloc([NB * 128], BF16)
            negmT = AR.alloc([NB, NQ], BF16)
            endX = AR.off
            AR.off = reg0
            sg2 = [AR.alloc([512], F32) for _ in range(2)]
            tmpm2 = [AR.alloc([512], F32) for _ in range(2)]
            mt = AR.alloc([nsub, 512], F32)
            mb = AR.alloc([nsub, D], BF16)
            mT = AR.alloc([8, 128], BF16)
            endY = AR.off
            AR.off = max(endX, endY)
            AR.peak = max(AR.peak, AR.off)
            XK = ["scores", "nmrow", "negmT"]
            YK = ["sg0", "sg1", "tmpm0", "tmpm1", "mT"] + ["mt%d" % i for i in range(nsub)] + ["mb%d" % i for i in range(nsub)]
            if not prompt:
                cstg = [AR.alloc([4, 128], F32) for _ in range(2)]
                cstb = [AR.alloc([4, 128], BF16) for _ in range(2)]
                bstg = [AR.alloc([4, 3, 64], F32) for _ in range(2)]
                bstb = [AR.alloc([4, 256], BF16) for _ in range(2)]

            ctr2 = {"stg": 0, "qn": 0, "tA": 0, "in": 0, "x": 0, "cs": 0, "rl": 0, "fz": 0, "junk": 0, "t1": 0, "sg": 0}

            def next_bank(nset):
                b = ctr["bank"] % nset
                ctr["bank"] += 1
                return b

            P.op("pool", [], ["av_sb"], lambda e: e.memset(av_sb[:, :, :, 128:129], 1.0))
            P.op("pool", [], ["bv_sb"], lambda e: e.memset(bv_sb[:, :, 64:65], 1.0))

            def transposes_bf16(src_ap_fn, nblk, Pin, reads, dst_write_keys, dst_ap, eng="act"):
                def f(e):
                    for bi in range(nblk):
                        ins = e.transpose(out=pTb[:, bi * Pin:(bi + 1) * Pin], in_=src_ap_fn(bi), identity=identb[0:Pin, 0:Pin])
                    return ins
                P.op("pe", reads + ["c_identb"], ["bk6"], f)
                src = pTb[:, 0:nblk * Pin].rearrange("p (b q) -> p b q", q=Pin)
                if eng == "act":
                    P.op("act", ["bk6"], dst_write_keys, lambda e: e.activation(out=dst_ap, in_=src, func=AF.Copy))
                else:
                    P.op(eng, ["bk6"], dst_write_keys, lambda e: e.tensor_copy(out=dst_ap, in_=src))

            def qknorm(bank, col0, ngroups, g_ap, out_ap, Pn_, extra_reads, out_keys):
                w = 64 * ngroups
                src = BK[bank][0:Pn_, col0:col0 + w]
                ss, ssk = smcol(ngroups)
                ss = ss[0:Pn_]
                ji = ctr2["junk"] % 2
                ctr2["junk"] += 1
                junk, jk = junk2[ji], "junk%d" % ji
                ti = ctr2["t1"] % 2
                ctr2["t1"] += 1
                t1, tk = t12[ti], "t1_%d" % ti
                P.op("act", ["bk%d" % bank], [jk], lambda e: e.activation(out=junk[0:Pn_, 0:w], in_=src, func=AF.Square))
                P.op("dve", [jk], ssk, lambda e: e.tensor_reduce(out=ss, in_=junk[0:Pn_, 0:w].rearrange("p (g d) -> p g d", d=64), axis=AX.X, op=ALU.add))
                rstd_from_ss(Pn_, ss, ssk, 1.0 / 64)
                P.op("dve", ["bk%d" % bank] + ssk, [tk], lambda e: e.tensor_tensor(
                    out=t1[0:Pn_, 0:w].rearrange("p (g d) -> p g d", d=64), in0=src.rearrange("p (g d) -> p g d", d=64),
                    in1=ss.unsqueeze(2).to_broadcast([Pn_, ngroups, 64]), op=ALU.mult))
                P.op("pool", [tk] + extra_reads, out_keys, lambda e: e.tensor_tensor(
                    out=out_ap.rearrange("p (g d) -> p g d", d=64), in0=t1[0:Pn_, 0:w].rearrange("p (g d) -> p g d", d=64),
                    in1=g_ap[0:Pn_].unsqueeze(1).to_broadcast([Pn_, ngroups, 64]), op=ALU.mult))

            def supertile(b, l, T):
                lam_init = 0.8 - 0.6 * math.exp(-0.3 * l)
                if prompt:
                    src = xp[b] if l == 0 else ybuf
                    dst = ybuf if l == 0 else yp[b]
                    srck = ("xp",) if l == 0 else ("ybuf",)
                    dstk = ("ybuf",) if l == 0 else ("yp",)
                    tok0 = T * 512
                    jb0 = T * 4
                    o_ak, o_av, o_bk, o_bv, o_bi = akp[l, b], avp[l, b], bkp[l, b], bvp[l, b], bip[l, b]
                else:
                    src = xs[b] if l == 0 else ybuf
                    dst = ybuf if l == 0 else ys[b]
                    srck = ("xs",) if l == 0 else ("ybuf",)
                    dstk = ("ybuf",) if l == 0 else ("ys",)
                    tok0 = 0
                    jb0 = NBC
                    o_ak, o_av, o_bk, o_bv, o_bi = aks[l, b], avs[l, b], bks[l, b], bvs[l, b], bis[l, b]
                nblk = jb0 + nsub

                def nk_of(j):
                    return 128 if prompt else (64 if j == NBC else 128)

                def imin(j):
                    return max(0, j - jb0) if prompt else 0

                def kind_of(j, i):
                    gt = jb0 + i
                    if j == gt:
                        return "diag"
                    if j == gt - 1:
                        return "near"
                    return "far"

                for i in range(nsub):
                    xi = ctr2["x"] % 2
                    ctr2["x"] += 1
                    xt = xin[xi][0:Pn, :]
                    r0 = tok0 + i * Pn
                    dma_in([srck + (r0 // 128,)] if l > 0 else [], ["xin%d" % xi], xt, src[r0:r0 + Pn, :])
                    ss, ssk = smcol(1)
                    ss = ss[0:Pn]
                    P.op("act", ["xin%d" % xi], ["xnb"] + ssk, lambda e, xt=xt, ss=ss: e.activation(out=xnb[0:Pn, :], in_=xt, func=AF.Square, accum_out=ss))
                    rstd_from_ss(Pn, ss, ssk, 1.0 / D)
                    P.op("act", ["xin%d" % xi] + ssk, ["xnb"], lambda e, xt=xt, ss=ss: e.activation(out=xnb[0:Pn, :], in_=xt, func=AF.Copy, scale=ss))
                    transposes_bf16(lambda bi: xnb[0:Pn, bi * 128:(bi + 1) * 128], 8, Pn, ["xnb"], ["xnT"], xnT[:, :, i * Pn:(i + 1) * Pn])

                if cfg.stop in ('pA', 'qA'):
                    return
                def project(kind, lidx, ncol, i, kch=8, lhs=None, lhs_keys=("xnT",), nset=6, ws=None):
                    bank = next_bank(nset)
                    lhs = xnT if lhs is None else lhs

                    def f(e):
                        for kc in range(kch):
                            ins = e.matmul(BK[bank][0:Pn, 0:ncol], lhsT=lhs[:, kc, i * Pn:(i + 1) * Pn], rhs=wslot[ws][:, kc, 0:ncol],
                                           start=(kc == 0), stop=(kc == kch - 1))
                        return ins
                    P.op("pe", list(lhs_keys) + ["wslot%d" % ws], ["bk%d" % bank], f)
                    return bank

                def out_rows(i):
                    r0 = tok0 + i * Pn
                    return slice(r0, r0 + Pn)

                kb = jb0

                def proj_step(g, ws, i):
                    if True:
                        bank = project("in", g, GCOLS[g], i, ws=ws)
                        bkk = "bk%d" % bank
                        rows = out_rows(i)
                        j = kb + i
                        if g == G_AQ:
                            q = ctr2["qn"] % 2
                            ctr2["qn"] += 1
                            qknorm(bank, 0, 8, gq_a[:, l, :], qn[q][0:Pn, :], Pn, ["gq_a%d" % l], ["qn%d" % q])
                            transposes_bf16(lambda bi, q=q: qn[q][0:Pn, bi * 128:(bi + 1) * 128], 4, Pn, ["qn%d" % q], ["aqT"], aqT[:, :, i * Pn:(i + 1) * Pn])
                        elif g == G_AK:
                            s_ = ctr2["stg"] % 2
                            ctr2["stg"] += 1
                            qknorm(bank, 0, 8, gk_a[:, l, :], stg[s_][0:Pn, :], Pn, ["gk_a%d" % l], ["stg%d" % s_])
                            dma_out(["stg%d" % s_], [], o_ak[rows, :], stg[s_][0:Pn, :])
                            q = ctr2["qn"] % 2
                            ctr2["qn"] += 1
                            P.op("pool", ["stg%d" % s_], ["qn%d" % q], lambda e, s_=s_, q=q: e.tensor_copy(out=qn[q][0:Pn, :], in_=stg[s_][0:Pn, :]))
                            if prompt:
                                transposes_bf16(lambda bi, q=q: qn[q][0:Pn, bi * 128:(bi + 1) * 128], 4, Pn, ["qn%d" % q], ["akT"],
                                                akT[:, :, j * 128:j * 128 + Pn])
                            else:
                                transposes_bf16(lambda bi, q=q: qn[q][0:Pn, bi * 128:(bi + 1) * 128], 4, Pn, ["qn%d" % q], ["akTown"],
                                                akTown[:, :, 0:Pn])
                        elif g == G_AV:
                            s_ = ctr2["stg"] % 2
                            ctr2["stg"] += 1
                            P.op("act", [bkk], ["stg%d" % s_], lambda e, s_=s_, bank=bank: e.activation(out=stg[s_][0:Pn, :], in_=BK[bank][0:Pn, :], func=AF.Copy))
                            dma_out(["stg%d" % s_], [], o_av[rows, :], stg[s_][0:Pn, :])
                            if prompt:
                                P.op("pool", ["stg%d" % s_], ["av_sb"], lambda e, s_=s_, j=j: e.tensor_copy(
                                    out=av_sb[0:Pn, j, :, 0:128], in_=stg[s_][0:Pn, :].rearrange("p (h d) -> p h d", d=128)))
                            else:
                                P.op("pool", ["stg%d" % s_], ["avown"], lambda e, s_=s_: e.tensor_copy(out=avown[0:Pn, :], in_=stg[s_][0:Pn, :]))
                        elif g in (G_AG, G_BG, G_CG):
                            n = {G_AG: 0, G_BG: 1, G_CG: 2}[g]
                            ti = ctr2["t1"] % 2
                            ctr2["t1"] += 1
                            t1, tk = t12[ti], "t1_%d" % ti
                            P.op("act", [bkk], [tk], lambda e, bank=bank, t1=t1: e.activation(out=t1[0:Pn, :], in_=BK[bank][0:Pn, :], func=AF.Silu))
                            if n == 0:
                                P.op("pool", [tk, "subln%d" % l], ["gate0"], lambda e, i=i, t1=t1: e.tensor_tensor(
                                    out=gates[0][0:Pn, i, :].rearrange("p (h d) -> p h d", d=128), in0=t1[0:Pn, :].rearrange("p (h d) -> p h d", d=128),
                                    in1=subln[0:Pn, l, :].unsqueeze(1).to_broadcast([Pn, 4, 128]), op=ALU.mult))
                            elif n == 2:
                                P.op("pool", [tk, "gn_c%d" % l], ["gate2"], lambda e, i=i, t1=t1: e.tensor_tensor(
                                    out=gates[2][0:Pn, i, :].rearrange("p (h d) -> p h d", d=128), in0=t1[0:Pn, :].rearrange("p (h d) -> p h d", d=128),
                                    in1=gn_c[0:Pn, l, :].unsqueeze(1).to_broadcast([Pn, 4, 128]), op=ALU.mult))
                            else:
                                P.op("pool", [tk], ["gate1"], lambda e, i=i, t1=t1: e.tensor_copy(out=gates[1][0:Pn, i, :], in_=t1[0:Pn, :]))
                        elif g == G_BQ:
                            q = ctr2["qn"] % 2
                            ctr2["qn"] += 1
                            qknorm(bank, 0, 8, gq_b[:, l, :], qn[q][0:Pn, :], Pn, ["gq_b%d" % l], ["qn%d" % q])
                            transposes_bf16(lambda bi, q=q: qn[q][0:Pn, bi * 128:(bi + 1) * 128], 4, Pn, ["qn%d" % q], ["bqT"], bqT[:, :, i * Pn:(i + 1) * Pn])
                        elif g == G_BM1:
                            s_ = ctr2["stg"] % 2
                            ctr2["stg"] += 1
                            qknorm(bank, 0, 2, gk_b[:, l, :], stg[s_][0:Pn, 0:128], Pn, ["gk_b%d" % l], ["stg%d" % s_])
                            P.op("act", [bkk, "stg%d" % s_], ["stg%d" % s_], lambda e, s_=s_, bank=bank: e.activation(
                                out=stg[s_][0:Pn, 128:512], in_=BK[bank][0:Pn, 128:512], func=AF.Copy))
                            dma_out(["stg%d" % s_], [], o_bk[rows, :], stg[s_][0:Pn, 0:64])
                            dma_out(["stg%d" % s_], [], o_bi[rows, :], stg[s_][0:Pn, 128:192])
                            q = ctr2["qn"] % 2
                            ctr2["qn"] += 1
                            P.op("pool", ["stg%d" % s_], ["qn%d" % q], lambda e, s_=s_, q=q: e.tensor_copy(out=qn[q][0:Pn, :], in_=stg[s_][0:Pn, :]))

                            def f(e, q=q):
                                for bi in range(4):
                                    ins = e.transpose(out=pTb[:, bi * Pn:(bi + 1) * Pn], in_=qn[q][0:Pn, bi * 128:(bi + 1) * 128], identity=identb[0:Pn, 0:Pn])
                                return ins
                            P.op("pe", ["qn%d" % q, "c_identb"], ["bk6"], f)
                            P.op("dve", ["bk6"], ["bkT"], lambda e, j=j: e.tensor_copy(out=bkT[:, j * 128:j * 128 + Pn], in_=pTb[:, 0:Pn]))
                            P.op("dve", ["bk6"], ["bkiT"], lambda e, j=j: e.tensor_copy(out=bkiT[:, j * 128:j * 128 + Pn], in_=pTb[:, Pn:2 * Pn]))
                            P.op("dve", ["bk6"], ["bqiT"], lambda e, i=i: e.tensor_copy(
                                out=bqiT[:, :, i * Pn:(i + 1) * Pn], in_=pTb[:, 2 * Pn:4 * Pn].rearrange("p (b q) -> p b q", q=Pn)))
                        elif g == G_BM2:
                            s_ = ctr2["stg"] % 2
                            ctr2["stg"] += 1
                            P.op("act", [bkk], ["stg%d" % s_], lambda e, s_=s_, bank=bank: e.activation(out=stg[s_][0:Pn, 0:68], in_=BK[bank][0:Pn, 0:68], func=AF.Copy))
                            dma_out(["stg%d" % s_], [], o_bv[rows, :], stg[s_][0:Pn, 0:64])
                            P.op("pool", ["stg%d" % s_], ["bv_sb"], lambda e, s_=s_, j=j: e.tensor_copy(out=bv_sb[0:Pn, j, 0:64], in_=stg[s_][0:Pn, 0:64]))
                            P.op("pool", ["stg%d" % s_], ["bwi"], lambda e, s_=s_, i=i: e.tensor_copy(out=bwi[0:Pn, i, :], in_=stg[s_][0:Pn, 64:68]))
                        elif g == G_CQK:
                            if prompt:
                                cs_, sn_ = cosP[0:Pn, jb0 + i, :], sinP[0:Pn, jb0 + i, :]
                            else:
                                cs_, sn_ = cosS[0:Pn, :], sinS[0:Pn, :]
                            zv = BK[bank][0:Pn, :].rearrange("p (g t d) -> p g t d", t=2, d=32)
                            x1, x2 = zv[:, :, 0, :], zv[:, :, 1, :]
                            rv = rotc[i][0:Pn, :].rearrange("p (g t d) -> p g t d", t=2, d=32)
                            cb = cs_.unsqueeze(1).to_broadcast([Pn, 8, 32])
                            sbc = sn_.unsqueeze(1).to_broadcast([Pn, 8, 32])
                            ra, rb_ = rtmp[0][0:Pn], rtmp[1][0:Pn]
                            P.op("dve", [bkk], ["rtmp0"], lambda e, x1=x1, cb=cb, ra=ra: e.tensor_tensor(out=ra, in0=x1, in1=cb, op=ALU.mult))
                            P.op("dve", [bkk], ["rtmp1"], lambda e, x2=x2, sbc=sbc, rb_=rb_: e.tensor_tensor(out=rb_, in0=x2, in1=sbc, op=ALU.mult))
                            P.op("pool", ["rtmp0", "rtmp1"], ["rotc"], lambda e, rv=rv, ra=ra, rb_=rb_: e.tensor_tensor(out=rv[:, :, 0, :], in0=ra, in1=rb_, op=ALU.subtract))
                            P.op("dve", [bkk, "rotc"], ["rtmp0"], lambda e, x1=x1, sbc=sbc, ra=ra: e.tensor_tensor(out=ra, in0=x1, in1=sbc, op=ALU.mult))
                            P.op("dve", [bkk, "rotc"], ["rtmp1"], lambda e, x2=x2, cb=cb, rb_=rb_: e.tensor_tensor(out=rb_, in0=x2, in1=cb, op=ALU.mult))
                            P.op("pool", ["rtmp0", "rtmp1"], ["rotc"], lambda e, rv=rv, ra=ra, rb_=rb_: e.tensor_tensor(out=rv[:, :, 1, :], in0=ra, in1=rb_, op=ALU.add))
                        elif g == G_CV:
                            P.op("act", [bkk], ["cvs"], lambda e, bank=bank, i=i: e.activation(out=cvs[i][0:Pn, :], in_=BK[bank][0:Pn, :], func=AF.Copy))


                dkt = dk128 if Pn == 128 else dk64
                gC = [float(gam[h] ** Pn) for h in range(4)]
                def retention(i):
                    rc = rotc[i]
                    rk_ = ["rotc"]
                    P.op("pool", rk_ + ["c_dq"], ["cqd"], lambda e, rc=rc: e.tensor_tensor(
                        out=cqd[0:Pn, :].rearrange("p (h d) -> p h d", d=64), in0=rc[0:Pn, 0:256].rearrange("p (h d) -> p h d", d=64),
                        in1=dq[0:Pn, :].unsqueeze(2).to_broadcast([Pn, 4, 64]), op=ALU.mult))
                    P.op("pool", rk_ + ["c_dk128" if Pn == 128 else "c_dk64"], ["ckd"], lambda e, rc=rc: e.tensor_tensor(
                        out=ckd[0:Pn, :].rearrange("p (h d) -> p h d", d=64), in0=rc[0:Pn, 256:512].rearrange("p (h d) -> p h d", d=64),
                        in1=dkt[0:Pn, :].unsqueeze(2).to_broadcast([Pn, 4, 64]), op=ALU.mult))
                    for ti, (srcap, sk_) in enumerate(((rc[0:Pn, 0:256], rk_), (cqd[0:Pn, :], ["cqd"]), (rc[0:Pn, 256:512], rk_))):
                        def f(e, srcap=srcap):
                            for bi in range(2):
                                ins = e.transpose(out=BK[7][:, bi * Pn:(bi + 1) * Pn], in_=srcap[:, bi * 128:(bi + 1) * 128], identity=identf[0:Pn, 0:Pn])
                            return ins
                        P.op("pe", sk_ + ["c_identf"], ["bk7"], f)
                        P.op("act", ["bk7"], ["cT%d" % ti], lambda e, ti=ti: e.activation(
                            out=cT[ti][:, :, 0:Pn], in_=BK[7][:, 0:2 * Pn].rearrange("p (b q) -> p b q", q=Pn), func=AF.Copy))
                    obank = next_bank(3)
                    for h in range(4):
                        hp, hi = (h % 2) * 64, h // 2
                        it = h % 2
                        P.op("pe", ["cT2", "cT0"], ["bk7"], lambda e, hp=hp, hi=hi: e.matmul(
                            BK[7][0:Pn, 0:Pn], lhsT=cT[2][hp:hp + 64, hi, 0:Pn], rhs=cT[0][hp:hp + 64, hi, 0:Pn], start=True, stop=True))
                        P.op("dve", ["bk7", "c_DT"], ["innerTm%d" % it], lambda e, h=h, it=it: e.tensor_tensor(
                            out=innerTm[it][0:Pn, 0:Pn], in0=BK[7][0:Pn, 0:Pn], in1=DT[0:Pn, h, 0:Pn], op=ALU.mult))

                        def f(e, h=h, hp=hp, hi=hi, it=it, i=i):
                            e.matmul(BK[obank][0:Pn, h * 128:(h + 1) * 128], lhsT=innerTm[it][0:Pn, 0:Pn], rhs=cvs[i][0:Pn, h * 128:(h + 1) * 128], start=True, stop=False)
                            return e.matmul(BK[obank][0:Pn, h * 128:(h + 1) * 128], lhsT=cT[1][:, hi, 0:Pn], rhs=state[:, h, :], start=False, stop=True)
                        P.op("pe", ["innerTm%d" % it, "cvs", "cT1", "state"], ["bk%d" % obank], f)
                    for h in range(4):
                        hp, hi = (h % 2) * 64, h // 2
                        P.op("pe", ["ckd", "cvs"], ["bk7"], lambda e, h=h, hi=hi, i=i: e.matmul(
                            BK[7][:, 0:128], lhsT=ckd[0:Pn, hi * 128:(hi + 1) * 128], rhs=cvs[i][0:Pn, h * 128:(h + 1) * 128], start=True, stop=True))
                        P.op("dve", ["bk7", "state"], ["state"], lambda e, h=h, hp=hp, hi=hi: e.scalar_tensor_tensor(
                            out=state[hp:hp + 64, h, :], in0=state[hp:hp + 64, h, :], scalar=gC[h], in1=BK[7][hp:hp + 64, 0:128], op0=ALU.mult, op1=ALU.add))
                    ss, ssk = smcol(4)
                    ss = ss[0:Pn]
                    for h in range(4):
                        P.op("act", ["bk%d" % obank], ["junk0", ssk[h]], lambda e, h=h, ss=ss: e.activation(
                            out=junk2[0][0:Pn, 0:128], in_=BK[obank][0:Pn, h * 128:(h + 1) * 128], func=AF.Square, accum_out=ss[:, h:h + 1]))
                    rstd_from_ss(Pn, ss, ssk, 1.0 / 128)
                    for h in range(4):
                        P.op("dve", ["bk%d" % obank, "gate2"] + ssk, ["brX"], lambda e, h=h, ss=ss, i=i: e.scalar_tensor_tensor(
                            out=brX[0:Pn, i, h * 128:(h + 1) * 128], in0=BK[obank][0:Pn, h * 128:(h + 1) * 128], scalar=ss[:, h:h + 1],
                            in1=gates[2][0:Pn, i, h * 128:(h + 1) * 128], op0=ALU.mult, op1=ALU.mult))
                    transposes_bf16(lambda bi, i=i: brX[0:Pn, i, bi * 128:(bi + 1) * 128], 4, Pn, ["brX"], ["brT2"], brT[:, 2, :, i * Pn:(i + 1) * Pn], eng="act")

                mask_from = nsub
                for i_ in range(nsub):
                    nkeys_ = (jb0 + i_ + 1) * 128 if prompt else PAST + 64
                    if nkeys_ > TOPK:
                        mask_from = min(mask_from, i_)

                def d_section():
                    P.fence(YK, XK)
                    if not prompt:
                        P.op("pool", [], ["negmT"], lambda e: e.memset(negmT[64:128, NB - 1, :], 0.0))
                    for i in range(nsub):
                        nkeys = (jb0 + i + 1) * 128 if prompt else PAST + 64
                        if nkeys <= TOPK:
                            continue
                        pass
                        for c0 in range(0, nkeys, 512):
                            cw = min(512, nkeys - c0)
                            for h in range(4):
                                hp, hi = (h % 2) * 64, h // 2
                                bank = 7
                                P.op("pe", ["bqiT", "bkiT"], ["bk%d" % bank], lambda e, bank=bank, hp=hp, hi=hi, c0=c0, cw=cw, i=i: e.matmul(
                                    BK[bank][0:Pn, 0:cw], lhsT=bqiT[hp:hp + 64, hi, i * Pn:(i + 1) * Pn], rhs=bkiT[hp:hp + 64, c0:c0 + cw], start=True, stop=True))
                                ri = ctr2["rl"] % 2
                                ctr2["rl"] += 1
                                P.op("act", ["bk%d" % bank], ["rl%d" % ri], lambda e, bank=bank, cw=cw, ri=ri: e.activation(out=rl[ri][0:Pn, 0:cw], in_=BK[bank][0:Pn, 0:cw], func=AF.Relu))
                                if h == 0:
                                    P.op("dve", ["rl%d" % ri, "bwi"], ["scores"], lambda e, c0=c0, cw=cw, i=i, ri=ri: e.tensor_scalar(
                                        out=scores[0:Pn, c0:c0 + cw], in0=rl[ri][0:Pn, 0:cw], scalar1=bwi[0:Pn, i, 0:1], scalar2=None, op0=ALU.mult))
                                else:
                                    P.op("dve", ["rl%d" % ri, "bwi", "scores"], ["scores"], lambda e, c0=c0, cw=cw, i=i, h=h, ri=ri: e.scalar_tensor_tensor(
                                        out=scores[0:Pn, c0:c0 + cw], in0=rl[ri][0:Pn, 0:cw], scalar=bwi[0:Pn, i, h:h + 1], in1=scores[0:Pn, c0:c0 + cw],
                                        op0=ALU.mult, op1=ALU.add))
                            yield
                        if prompt:
                            P.op("dve", ["scores"], ["scores"], lambda e, nkeys=nkeys: e.memset(scores[0:64, nkeys - 64:nkeys], NEG))
                        sc = scores[0:Pn, 0:nkeys]
                        for r in range(TOPK // 8):
                            P.op("dve", ["scores"], ["m8"], lambda e, sc=sc: e.max(out=m8[0:Pn, :], in_=sc))
                            P.op("dve", ["scores", "m8"], ["scores"], lambda e, sc=sc: e.match_replace(out=sc, in_to_replace=m8[0:Pn, :], in_values=sc, imm_value=NEG))
                            yield
                        P.op("dve", ["scores"], ["nmrow"], lambda e, sc=sc, nkeys=nkeys: e.tensor_scalar(
                            out=nmrow[0:Pn, 0:nkeys], in0=sc, scalar1=-1.0e38, scalar2=-30000.0, op0=ALU.is_gt, op1=ALU.mult))
                        nb_i = (nkeys + 127) // 128
                        per = 1024 // Pn
                        for j0 in range(0, nb_i, per):
                            nb_ = min(per, nb_i - j0)

                            def f(e, j0=j0, nb_=nb_, nkeys=nkeys):
                                for bj in range(nb_):
                                    kk = min(128, nkeys - (j0 + bj) * 128)
                                    ins = e.transpose(out=pTb[0:kk, bj * Pn:(bj + 1) * Pn], in_=nmrow[0:Pn, (j0 + bj) * 128:(j0 + bj) * 128 + kk], identity=identb[0:Pn, 0:Pn])
                                return ins
                            P.op("pe", ["nmrow", "c_identb"], ["bk6"], f)
                            rr = 128
                            if nkeys - (j0 + nb_ - 1) * 128 < 128:
                                assert nb_ == 1
                                rr = nkeys - j0 * 128
                            P.op("act", ["bk6"], ["negmT"], lambda e, j0=j0, nb_=nb_, i=i, rr=rr: e.activation(
                                out=negmT[0:rr, j0:j0 + nb_, i * Pn:(i + 1) * Pn], in_=pTb[0:rr, 0:nb_ * Pn].rearrange("p (b q) -> p b q", q=Pn), func=AF.Copy))
                        yield


                _gen = [None]

                def pump(n):
                    for _ in range(n):
                        if _gen[0] is not None and next(_gen[0], "done") == "done":
                            _gen[0] = None

                for g in (G_BM1, G_BM2):
                    ws = wload("in", l, g)
                    for i in range(nsub):
                        proj_step(g, ws, i)
                _gen[0] = d_section()
                for g in (G_AQ, G_AK, G_AV, G_AG, G_BQ, G_BG, G_CG):
                    ws = wload("in", l, g)
                    for i in range(nsub):
                        proj_step(g, ws, i)
                        pump(4)
                ws_q = wload("in", l, G_CQK)
                ws_v = wload("in", l, G_CV)
                for i in range(nsub):
                    proj_step(G_CQK, ws_q, i)
                    proj_step(G_CV, ws_v, i)
                    retention(i)
                    pump(4)
                if cfg.stop in ('pD', 'qD'):
                    return
                def attention(steps, s_emit, exp_bias, pv_emit, npump=0):
                    LA = 2
                    n = len(steps)
                    info = {}
                    for k in range(n + LA):
                        if npump:
                            pump(npump)
                        if k < n:
                            bank = next_bank(3)
                            info[k] = bank
                            s_emit(k, steps[k], bank)
                        kk = k - LA
                        if kk >= 0:
                            bank = info[kk]
                            j = steps[kk][0]
                            nk = nk_of(j)
                            q0 = imin(j) * Pn
                            pi = ctr["pt"] % NPT
                            ctr["pt"] += 1
                            bias_ap = exp_bias(steps[kk])
                            P.op("act", ["bk%d" % bank, "farb"], ["PT%d" % pi], lambda e, bank=bank, nk=nk, q0=q0, pi=pi, bias_ap=bias_ap: e.activation(
                                out=PT[pi][0:nk, q0:NQ], in_=BK[bank][0:nk, q0:NQ], func=AF.Exp, bias=bias_ap[0:nk], scale=0.125))
                            pv_emit(kk, steps[kk], pi)

                acc_pos_A = {}
                for c in range(2):
                    for i in range(nsub):
                        idx = c * nsub + i
                        acc_pos_A[(c, i)] = (3 + idx // 3, (idx % 3) * 130)
                for h in range(4):
                    hs = h if prompt else 0
                    if not prompt:
                        prefill_A(b, l, h)
                    started = set()

                    def s_emit(k, stp, bank, h=h, hs=hs):
                        j, c = stp
                        nk = nk_of(j)
                        q0 = imin(j) * Pn
                        subs = [(i, kind_of(j, i)) for i in range(imin(j), nsub) if kind_of(j, i) != "far"]

                        def f(e):
                            ins = e.matmul(BK[bank][0:nk, q0:NQ], lhsT=akT[c * 64:(c + 1) * 64, hs, j * 128:j * 128 + nk], rhs=aqT[c * 64:(c + 1) * 64, h, q0:NQ],
                                           start=True, stop=(len(subs) == 0))
                            for si, (i, kd) in enumerate(subs):
                                ins = e.matmul(BK[bank][0:nk, i * Pn:(i + 1) * Pn], lhsT=identb[:, 0:nk], rhs=biasT[:, 0 if kd == "diag" else 1, h, 0:Pn],
                                               start=False, stop=(si == len(subs) - 1))
                            return ins
                        P.op("pe", ["akT", "aqT", "biasT", "c_identb"], ["bk%d" % bank], f)

                    def pv_emit(kk, stp, pi, h=h, hs=hs, started=started):
                        j, c = stp
                        nk = nk_of(j)

                        def f(e):
                            for i in range(imin(j), nsub):
                                ob, off = acc_pos_A[(c, i)]
                                st_ = ob not in started
                                started.add(ob)
                                last = (j == jb0 + i) if prompt else (j == NBC)
                                ins = e.matmul(BK[ob][0:Pn, off:off + 129], lhsT=PT[pi][0:nk, i * Pn:(i + 1) * Pn], rhs=av_sb[0:nk, j, hs, 0:129],
                                               start=st_, stop=last, skip_group_check=True)
                            return ins
                        P.op("pe", ["PT%d" % pi, "av_sb"], ["bk3", "bk4", "bk5"], f)

                    steps = [(j, c) for j in range(nblk) for c in range(2)]
                    attention(steps, s_emit, lambda stp, h=h: farb[:, l, h:h + 1], pv_emit, npump=1)
                    for i in range(nsub):
                        ob0, of0 = acc_pos_A[(0, i)]
                        ob1, of1 = acc_pos_A[(1, i)]
                        fi = ctr2["fz"] % 4
                        ctr2["fz"] += 1
                        z = fz[fi][0:Pn]
                        zk = "fz%d" % fi
                        P.op("act", ["bk%d" % ob0], [zk], lambda e, z=z, ob0=ob0, of0=of0: e.activation(out=z[:, 0:1], in_=BK[ob0][0:Pn, of0 + 128:of0 + 129], func=AF.Ln))
                        P.op("act", ["bk%d" % ob1, zk], [zk], lambda e, z=z, ob1=ob1, of1=of1: e.activation(out=z[:, 1:2], in_=BK[ob1][0:Pn, of1 + 128:of1 + 129], func=AF.Ln))
                        P.op("act", [zk], [zk], lambda e, z=z: e.activation(out=z[:, 0:2], in_=z[:, 0:2], func=AF.Exp, scale=-1.0))
                        P.op("pool", [zk, "neglam"], [zk], lambda e, z=z: e.tensor_scalar(out=z[:, 1:2], in0=z[:, 1:2], scalar1=neglam[0:Pn, l:l + 1], scalar2=None, op0=ALU.mult))
                        ta = ctr2["tA"] % 2
                        ctr2["tA"] += 1
                        P.op("act", ["bk%d" % ob0, zk], ["tA%d" % ta], lambda e, z=z, ob0=ob0, of0=of0, ta=ta: e.activation(
                            out=tA[ta][0:Pn, :], in_=BK[ob0][0:Pn, of0:of0 + 128], func=AF.Copy, scale=z[:, 0:1]))
                        P.op("act", ["bk%d" % ob1, zk], ["tB%d" % ta], lambda e, z=z, ob1=ob1, of1=of1, ta=ta: e.activation(
                            out=tB[ta][0:Pn, :], in_=BK[ob1][0:Pn, of1:of1 + 128], func=AF.Copy, scale=z[:, 1:2]))
                        P.op("pool", ["tA%d" % ta, "tB%d" % ta], ["tA%d" % ta], lambda e, ta=ta: e.tensor_tensor(out=tA[ta][0:Pn, :], in0=tA[ta][0:Pn, :], in1=tB[ta][0:Pn, :], op=ALU.add))
                        P.op("act", ["tA%d" % ta, zk], ["tB%d" % ta, zk], lambda e, z=z, ta=ta: e.activation(
                            out=tB[ta][0:Pn, :], in_=tA[ta][0:Pn, :], func=AF.Square, accum_out=z[:, 2:3]))
                        P.op("act", [zk], [zk], lambda e, z=z: e.activation(out=z[:, 2:3], in_=z[:, 2:3], func=AF.Ln, scale=1.0 / 128, bias=epsc[0:Pn, :]))
                        P.op("act", [zk], [zk], lambda e, z=z: e.activation(out=z[:, 2:3], in_=z[:, 2:3], func=AF.Exp, scale=-0.5))
                        P.op("pool", ["tA%d" % ta, zk], ["tA%d" % ta], lambda e, z=z, ta=ta: e.tensor_scalar(
                            out=tA[ta][0:Pn, :], in0=tA[ta][0:Pn, :], scalar1=z[:, 2:3], scalar2=None, op0=ALU.mult))
                        P.op("pool", ["tA%d" % ta, "gate0"], ["brX"], lambda e, ta=ta, i=i, h=h: e.tensor_tensor(
                            out=brX[0:Pn, i, h * 128:(h + 1) * 128], in0=tA[ta][0:Pn, :], in1=gates[0][0:Pn, i, h * 128:(h + 1) * 128], op=ALU.mult))
                for i in range(nsub):
                    transposes_bf16(lambda bi, i=i: brX[0:Pn, i, bi * 128:(bi + 1) * 128], 4, Pn, ["brX"], ["brT0"], brT[:, 0, :, i * Pn:(i + 1) * Pn], eng="act")

                if cfg.stop in ('pE', 'qE'):
                    return
                pump(100000)
                for h in range(8):
                    hp, hi = (h % 2) * 64, h // 2
                    ob = 3 + (h % 3)

                    def s_emit(k, stp, bank, h=h, hp=hp, hi=hi):
                        j = stp[0]
                        nk = nk_of(j)
                        q0 = imin(j) * Pn
                        subs = [(i, kind_of(j, i)) for i in range(imin(j), nsub) if kind_of(j, i) != "far"]
                        m0 = max(mask_from, imin(j)) * Pn
                        has_mask = m0 < NQ

                        def f(e):
                            ins = e.matmul(BK[bank][0:nk, q0:NQ], lhsT=bkT[hp:hp + 64, j * 128:j * 128 + nk], rhs=bqT[hp:hp + 64, hi, q0:NQ],
                                           start=True, stop=(len(subs) == 0 and not has_mask))
                            if has_mask:
                                ins = e.matmul(BK[bank][0:nk, m0:NQ], lhsT=identb[:, 0:nk], rhs=negmT[:, j, m0:NQ], start=False, stop=(len(subs) == 0))
                            for si, (i, kd) in enumerate(subs):
                                ins = e.matmul(BK[bank][0:nk, i * Pn:(i + 1) * Pn], lhsT=identb[:, 0:nk], rhs=biasT[:, 0 if kd == "diag" else 1, 4 + h, 0:Pn],
                                               start=False, stop=(si == len(subs) - 1))
                            return ins
                        P.op("pe", ["bkT", "bqT", "biasT", "negmT", "c_identb"], ["bk%d" % bank], f)

                    def pv_emit(kk, stp, pi, h=h, ob=ob):
                        j = stp[0]
                        nk = nk_of(j)

                        def f(e):
                            for i in range(imin(j), nsub):
                                last = (j == jb0 + i) if prompt else (j == NBC)
                                ins = e.matmul(BK[ob][0:Pn, i * 66:i * 66 + 65], lhsT=PT[pi][0:nk, i * Pn:(i + 1) * Pn], rhs=bv_sb[0:nk, j, 0:65],
                                               start=(j == 0 and i == 0), stop=last, skip_group_check=True)
                            return ins
                        P.op("pe", ["PT%d" % pi, "bv_sb"], ["bk%d" % ob], f)

                    steps = [(j,) for j in range(nblk)]
                    attention(steps, s_emit, lambda stp, h=h: farb[:, l, 4 + h:5 + h], pv_emit)
                    for i in range(nsub):
                        fi = ctr2["fz"] % 4
                        ctr2["fz"] += 1
                        z = fz[fi][0:Pn]
                        zk = "fz%d" % fi
                        ta = ctr2["tA"] % 2
                        ctr2["tA"] += 1
                        P.op("act", ["bk%d" % ob], [zk], lambda e, z=z, ob=ob, i=i: e.activation(out=z[:, 0:1], in_=BK[ob][0:Pn, i * 66 + 64:i * 66 + 65], func=AF.Ln))
                        P.op("act", [zk], [zk], lambda e, z=z: e.activation(out=z[:, 0:1], in_=z[:, 0:1], func=AF.Exp, scale=-1.0))
                        P.op("act", ["bk%d" % ob, zk], ["tA%d" % ta], lambda e, z=z, ob=ob, i=i, ta=ta: e.activation(
                            out=tA[ta][0:Pn, 0:64], in_=BK[ob][0:Pn, i * 66:i * 66 + 64], func=AF.Copy, scale=z[:, 0:1]))
                        P.op("pool", ["tA%d" % ta, "gate1"], ["brX"], lambda e, ta=ta, i=i, h=h: e.tensor_tensor(
                            out=brX[0:Pn, i, h * 64:(h + 1) * 64], in0=tA[ta][0:Pn, 0:64], in1=gates[1][0:Pn, i, h * 64:(h + 1) * 64], op=ALU.mult))
                for i in range(nsub):
                    transposes_bf16(lambda bi, i=i: brX[0:Pn, i, bi * 128:(bi + 1) * 128], 4, Pn, ["brX"], ["brT1"], brT[:, 1, :, i * Pn:(i + 1) * Pn], eng="act")

                if cfg.stop in ('pF', 'qF'):
                    return
                P.fence(XK, YK)
                for hf in range(2):
                    for n in range(3):
                        ws_g = wload("in", l, G_MG0 + n * 2 + hf)
                        ws_b = wload("br", l, n * 2 + hf)
                        for i in range(nsub):
                            bg = project("in", None, 512, i, ws=ws_g)
                            si_ = ctr2["sg"] % 2
                            ctr2["sg"] += 1
                            sg, sgk, tmpm, tmk = sg2[si_], "sg%d" % si_, tmpm2[si_], "tmpm%d" % si_
                            P.op("act", ["bk%d" % bg], [sgk], lambda e, bg=bg, sg=sg: e.activation(out=sg[0:Pn, :], in_=BK[bg][0:Pn, :], func=AF.Sigmoid))
                            bp = project("br", None, 512, i, kch=4, lhs=brT[:, n], lhs_keys=("brT%d" % n,), ws=ws_b)
                            if n == 0:
                                P.op("dve", ["bk%d" % bp, sgk], ["mt%d" % i], lambda e, bp=bp, i=i, sg=sg: e.tensor_tensor(
                                    out=mt[0:Pn, i, :], in0=BK[bp][0:Pn, :], in1=sg[0:Pn, :], op=ALU.mult))
                            else:
                                P.op("dve", ["bk%d" % bp, sgk], [tmk], lambda e, bp=bp, sg=sg, tmpm=tmpm: e.tensor_tensor(out=tmpm[0:Pn, :], in0=BK[bp][0:Pn, :], in1=sg[0:Pn, :], op=ALU.mult))
                                P.op("pool", [tmk, "mt%d" % i], ["mt%d" % i], lambda e, i=i, tmpm=tmpm: e.tensor_tensor(
                                    out=mt[0:Pn, i, :], in0=mt[0:Pn, i, :], in1=tmpm[0:Pn, :], op=ALU.add))
                    for i in range(nsub):
                        P.op("act", ["mt%d" % i], ["mb%d" % i], lambda e, i=i, hf=hf: e.activation(out=mb[0:Pn, i, hf * 512:(hf + 1) * 512], in_=mt[0:Pn, i, :], func=AF.Copy))
                ws_os = [wload("out", l, 0), wload("out", l, 1)]
                for i in range(nsub):
                    transposes_bf16(lambda bi, i=i: mb[0:Pn, i, bi * 128:(bi + 1) * 128], 8, Pn, ["mb%d" % i], ["mT"], mT[:, :, 0:Pn])
                    xi = ctr2["x"] % 2
                    ctr2["x"] += 1
                    r0 = tok0 + i * Pn
                    dma_in([srck + (r0 // 128,)] if l > 0 else [], ["xin%d" % xi], xin[xi][0:Pn, :], src[r0:r0 + Pn, :])
                    for hf in range(2):
                        ws_o = ws_os[hf]
                        bo = next_bank(6)

                        def f(e, bo=bo, ws_o=ws_o):
                            for kc in range(8):
                                ins = e.matmul(BK[bo][0:Pn, :], lhsT=mT[:, kc, 0:Pn], rhs=wslot[ws_o][:, kc, :], start=(kc == 0), stop=(kc == 7))
                            return ins
                        P.op("pe", ["mT", "wslot%d" % ws_o], ["bk%d" % bo], f)
                        P.op("dve", ["bk%d" % bo, "xin%d" % xi], ["xin%d" % xi], lambda e, bo=bo, xi=xi, hf=hf: e.tensor_tensor(
                            out=xin[xi][0:Pn, hf * 512:(hf + 1) * 512], in0=BK[bo][0:Pn, :], in1=xin[xi][0:Pn, hf * 512:(hf + 1) * 512], op=ALU.add))
                    dma_out(["xin%d" % xi], [dstk + (r0 // 128,)] if l == 0 else [], dst[r0:r0 + Pn, :], xin[xi][0:Pn, :])

            if not prompt:
                akTown = AR.alloc([4, 64], BF16)
                avown = AR.alloc([512], F32)

            def prefill_B(b, l):
                for j0 in range(0, NBC, 4):
                    nb_ = min(4, NBC - j0)
                    si = ctr2["cs"] % 2
                    ctr2["cs"] += 1
                    for t_, srcc in enumerate((cbk, cbi, cbv)):
                        dma_in([], ["bstg%d_%d" % (si, t_)], bstg[si][:, 0:nb_, t_, :],
                               srcc[l, b, j0 * 128:(j0 + nb_) * 128, :].rearrange("(j p) d -> p j d", p=128))
                    rk = ["bstg%d_%d" % (si, t_) for t_ in range(3)]
                    if cfg.lvl < 2:
                        continue
                    for rep in range(2):
                        P.op("pool", rk, ["bstb%d" % si], lambda e, si=si, rep=rep, nb_=nb_: e.tensor_copy(
                            out=bstb[si][:, 0:nb_, :].rearrange("p j (t r d) -> p j t r d", t=2, r=2)[:, :, :, rep, :], in_=bstg[si][:, 0:nb_, 0:2, :]))
                    P.op("pool", rk, ["bv_sb"], lambda e, si=si, nb_=nb_, j0=j0: e.tensor_copy(out=bv_sb[:, j0:j0 + nb_, 0:64], in_=bstg[si][:, 0:nb_, 2, :]))
                    if cfg.lvl < 3:
                        continue

                    def f(e, si=si, nb_=nb_):
                        for bj in range(nb_):
                            for t_ in range(2):
                                ins = e.transpose(out=pTb[:, (bj * 2 + t_) * 128:(bj * 2 + t_ + 1) * 128], in_=bstb[si][:, bj, t_ * 128:(t_ + 1) * 128], identity=identb[:, :])
                        return ins
                    P.op("pe", ["bstb%d" % si, "c_identb"], ["bk6"], f)
                    pv_ = pTb[:, 0:nb_ * 256].rearrange("p (j t q) -> p j t q", t=2, q=128)
                    if cfg.lvl < 4:
                        continue
                    P.op("dve", ["bk6"], ["bkT"], lambda e, pv_=pv_, j0=j0, nb_=nb_: e.tensor_copy(
                        out=bkT[:, j0 * 128:(j0 + nb_) * 128].rearrange("p (j q) -> p j q", q=128), in_=pv_[:, :, 0, :]))
                    if cfg.lvl < 5:
                        continue
                    P.op("dve", ["bk6"], ["bkiT"], lambda e, pv_=pv_, j0=j0, nb_=nb_: e.tensor_copy(
                        out=bkiT[:, j0 * 128:(j0 + nb_) * 128].rearrange("p (j q) -> p j q", q=128), in_=pv_[:, :, 1, :]))

            def prefill_A(b, l, h):
                for j0 in range(0, NBC, 4):
                    nb_ = min(4, NBC - j0)
                    si = ctr2["cs"] % 2
                    ctr2["cs"] += 1
                    dma_in([], ["cstg%d" % si], cstg[si][:, 0:nb_, :], cak[l, b, j0 * 128:(j0 + nb_) * 128, h * 128:(h + 1) * 128].rearrange("(j p) d -> p j d", p=128))
                    P.op("pool", ["cstg%d" % si], ["cstb%d" % si], lambda e, si=si, nb_=nb_: e.tensor_copy(out=cstb[si][:, 0:nb_, :], in_=cstg[si][:, 0:nb_, :]))

                    def f(e, si=si, nb_=nb_):
                        for bj in range(nb_):
                            ins = e.transpose(out=pTb[:, bj * 128:(bj + 1) * 128], in_=cstb[si][:, bj, :], identity=identb[:, :])
                        return ins
                    P.op("pe", ["cstb%d" % si, "c_identb"], ["bk6"], f)
                    P.op("dve", ["bk6"], ["akT"], lambda e, j0=j0, nb_=nb_: e.tensor_copy(out=akT[:, 0, j0 * 128:(j0 + nb_) * 128], in_=pTb[:, 0:nb_ * 128]))
                    si = ctr2["cs"] % 2
                    ctr2["cs"] += 1
                    dma_in([], ["cstg%d" % si], cstg[si][:, 0:nb_, :], cav[l, b, j0 * 128:(j0 + nb_) * 128, h * 128:(h + 1) * 128].rearrange("(j p) d -> p j d", p=128))
                    P.op("act", ["cstg%d" % si], ["av_sb"], lambda e, si=si, j0=j0, nb_=nb_: e.activation(out=av_sb[:, j0:j0 + nb_, 0, 0:128], in_=cstg[si][:, 0:nb_, :], func=AF.Copy))
                P.op("dve", ["akTown"], ["akT"], lambda e, h=h: e.tensor_copy(out=akT[:, 0, NBC * 128:NBC * 128 + 64], in_=akTown[:, h, :]))
                P.op("dve", ["avown"], ["av_sb"], lambda e, h=h: e.tensor_copy(out=av_sb[0:64, NBC, 0, 0:128], in_=avown[0:64, h * 128:(h + 1) * 128]))

            for b in range(nseq):
                for l in range(2):
                    if prompt:
                        P.op("pool", [], ["state"], lambda e: e.memset(state[:, :, :], 0.0))
                        for T in range(S // 512):
                            supertile(b, l, T)
                            if cfg.stop in ('pA', 'pB', 'pC', 'pD', 'pE', 'pF', 'pG'):
                                break
                        sc_out = scp[l, b]
                    else:
                        P.op("pool", [], ["state"], lambda e: e.memset(state[:, :, :], 0.0))
                        for h_ in range(4):
                            dma_in([], ["state"], state[(h_ % 2) * 64:(h_ % 2) * 64 + 64, h_, :], stc[l, b, h_])
                        if cfg.lvl >= 1:
                            prefill_B(b, l)
                        if cfg.stop == "qP":
                            break
                        supertile(b, l, 0)
                        if cfg.stop in ('qA', 'qB', 'qC', 'qD', 'qE', 'qF', 'qG'):
                            break
                        sc_out = scs[l, b]
                    for h_ in range(4):
                        dma_out(["state"], [], sc_out[h_], state[(h_ % 2) * 64:(h_ % 2) * 64 + 64, h_, :])

        def dbg(name, ap, keys):
            pass

        if cfg.stop not in ("setup", "conv", "s1", "s2", "s3"):
            if cfg.stop is None or cfg.stop[0] == "p":
                run_phase("p")
            if cfg.stop is None or cfg.stop[0] == "q":
                run_phase("s")
        P.emit(st)
        print("arena peak bytes", AR.peak, "of", ARENA_BYTES, "instr counts", {e: len(v) for e, v in P.ops.items()}, flush=True)
    return nc


SEG = [512, 512, 512, 512, 512, 64, 64, 256, 64, 4, 512, 256, 256, 512, 512, 3072]


def group_w_in(w_in):
    off = np.concatenate([[0], np.cumsum(SEG)])
    seg = lambda i: w_in[:, :, off[i]:off[i + 1]]
    aq, ak, av, ag, bq, bk, bv, bqi, bki, bwi, bg, cq, ck, cv, cg, mg = [seg(i) for i in range(16)]
    out = np.zeros((2, NGRP, D, 512), np.float32)
    out[:, G_AQ], out[:, G_AK], out[:, G_AV], out[:, G_AG], out[:, G_BQ] = aq, ak, av, ag, bq
    out[:, G_BM1] = np.concatenate([bk, bk, bki, bki, bqi], axis=-1)
    out[:, G_BM2, :, 0:64] = bv
    out[:, G_BM2, :, 64:68] = bwi
    out[:, G_BG] = bg
    out[:, G_CQK] = np.concatenate([cq, ck], axis=-1)
    out[:, G_CV], out[:, G_CG] = cv, cg
    for i in range(6):
        out[:, G_MG0 + i] = mg[:, :, i * 512:(i + 1) * 512]
    return out


_CACHE = {}


def run(cfg, inputs, n_cores):
    key = (cfg.NBP, cfg.S, cfg.NBS, cfg.PAST)
    if key not in _CACHE:
        _CACHE[key] = build(cfg)
    nc = _CACHE[key]
    consts, _ = make_consts(cfg)
    f = lambda a: np.ascontiguousarray(np.asarray(a, dtype=np.float32))
    wing = group_w_in(f(inputs["w_in"]))
    ngT = np.ascontiguousarray(f(inputs["norm_g"]).reshape(2, 8, 128).transpose(0, 2, 1))
    shared = {
        "relb": f(inputs["rel_bias"]), "ngT": ngT, "wing": wing, "aqkg": f(inputs["a_qk_g"]), "alam": f(inputs["a_lambda"]),
        "asub": f(inputs["a_subln_g"]), "bqkg": f(inputs["b_qk_g"]), "cgn": f(inputs["c_gn_g"]), "wbr": f(inputs["w_branch"]),
        "wout": f(inputs["w_out"]),
    }
    for k, v in consts.items():
        shared["c_" + k] = np.ascontiguousarray(v)
    NBP, NBS = cfg.NBP, cfg.NBS
    xp_, xs_ = f(inputs["x_prompt"]), f(inputs["x_sample"])
    big = {k: np.asarray(inputs[k], dtype=np.float32) for k in ("cache_a_k", "cache_a_v", "cache_b_k", "cache_b_v", "cache_b_kidx", "state_c")}
    in_maps = []
    for c in range(n_cores):
        m = dict(shared)
        m["xp"] = np.ascontiguousarray(xp_[c * NBP:(c + 1) * NBP]) if NBP else np.zeros((1, cfg.S, D), np.float32)
        sl = slice(c * NBS, (c + 1) * NBS)
        m["xs"] = np.ascontiguousarray(xs_[sl])
        m["cak"] = np.ascontiguousarray(big["cache_a_k"][:, sl]).reshape(2, NBS, cfg.PAST, 512)
        m["cav"] = np.ascontiguousarray(big["cache_a_v"][:, sl]).reshape(2, NBS, cfg.PAST, 512)
        m["cbk"] = np.ascontiguousarray(big["cache_b_k"][:, sl])
        m["cbv"] = np.ascontiguousarray(big["cache_b_v"][:, sl])
        m["cbi"] = np.ascontiguousarray(big["cache_b_kidx"][:, sl])
        m["stc"] = np.ascontiguousarray(big["state_c"][:, sl])
        in_maps.append(m)
    res = run_bass_kernel_spmd(nc, in_maps, core_ids=list(range(n_cores)))
    R = res.results
    cat = lambda name, ax: np.concatenate([np.asarray(r[name], dtype=np.float32) for r in R], axis=ax)
    Bp, Bs = NBP * n_cores, NBS * n_cores
    outs = (
        cat("yp", 0), cat("ys", 0),
        cat("akp", 1).reshape(2, Bp, cfg.S, 4, 2, 64), cat("avp", 1).reshape(2, Bp, cfg.S, 4, 128),
        cat("bkp", 1), cat("bvp", 1), cat("bip", 1), cat("scp", 1),
        cat("aks", 1).reshape(2, Bs, 64, 4, 2, 64), cat("avs", 1).reshape(2, Bs, 64, 4, 128),
        cat("bks", 1), cat("bvs", 1), cat("bis", 1), cat("scs", 1),
    )
    return outs


def kernel(**inputs):
    cfg = Cfg()
    return run(cfg, inputs, 8)
```
